# Optimizing a Trainium2 kernel written in Bass

```python
import math
import jax, jax.numpy as jnp
from jax import lax
import numpy as np

D_MODEL = 1024
BATCH = 4
SEQ = 4096
DEPTH = 2

N_MIXERS = 2
N_A_LAYERS = (DEPTH + 1) // 2
N_B_LAYERS = DEPTH // 2
A_INNER = 3 * D_MODEL
A_CHUNK = 128
A_GROUPS = 8
A_GROUP_W = A_INNER // A_GROUPS
B_HEADS = 16
B_HEAD_DIM = 64
B_V_DIM = 64
B_KV_LATENT = 256
B_IDX_HEADS = 8
B_IDX_DIM = 64
B_TOPK_MAX = 256
B_QBLK = 128
B_PROJ = B_HEADS * B_HEAD_DIM + B_KV_LATENT + B_IDX_HEADS * B_IDX_DIM + B_IDX_DIM + B_IDX_HEADS
REL_BUCKETS = 32
REL_MAX_DIST = 128
N_EXPERTS = 16
N_GROUPS = 4
EXPERTS_PER_GROUP = N_EXPERTS // N_GROUPS
TOP_K = 2
D_EXPERT = 3584
MOE_BLOCK = 128
EPS = 1e-6

kernel_name = "hybrid_gmlp_dsa_grouped_moe_adaln"


def rms_norm(x, g):
    xf = x.astype(jnp.float32)
    y = xf * lax.rsqrt(jnp.mean(xf * xf, axis=-1, keepdims=True) + EPS)
    return (y * g.astype(jnp.float32)).astype(x.dtype)


def layer_norm(x, g, b):
    xf = x.astype(jnp.float32)
    mu = jnp.mean(xf, axis=-1, keepdims=True)
    var = jnp.mean(jnp.square(xf - mu), axis=-1, keepdims=True)
    y = (xf - mu) * lax.rsqrt(var + EPS)
    return (y * g.astype(jnp.float32) + b.astype(jnp.float32)).astype(x.dtype)


def t5_bucket(dist):
    n = jnp.maximum(dist, 0)
    exact = REL_BUCKETS // 2
    nf = jnp.maximum(n, 1).astype(jnp.float32)
    large = exact + (jnp.log(nf / exact) / math.log(REL_MAX_DIST / exact)
                     * (REL_BUCKETS - exact)).astype(jnp.int32)
    large = jnp.minimum(large, REL_BUCKETS - 1)
    return jnp.where(n < exact, n, large)


def gmlp_mixer(h, w_in, ln_g, ln_b, w_sp, b_sp, w_out):
    bsz, L, _ = h.shape
    z = jax.nn.gelu(h @ w_in)
    u, v = jnp.split(z, 2, axis=-1)
    v = layer_norm(v, ln_g, ln_b)
    nc = L // A_CHUNK
    v = v.reshape(bsz, nc, A_CHUNK, A_GROUPS, A_GROUP_W)
    causal = jnp.tril(jnp.ones((A_CHUNK, A_CHUNK), dtype=w_sp.dtype))
    ws = w_sp * causal
    fv = jnp.einsum('gts,bnsgc->bntgc', ws, v) + b_sp.T[None, None, :, :, None]
    s = u * fv.reshape(bsz, L, A_INNER)
    return s @ w_out


def dsa_mixer(h, w_in, kv_norm_g, w_uk, w_uv, kidx_g, w_out, rel_bias):
    bsz, L, _ = h.shape
    proj = h @ w_in
    o1 = B_HEADS * B_HEAD_DIM
    o2 = o1 + B_KV_LATENT
    o3 = o2 + B_IDX_HEADS * B_IDX_DIM
    o4 = o3 + B_IDX_DIM
    q, c_kv, q_i, k_i, w_i = jnp.split(proj, [o1, o2, o3, o4], axis=-1)
    q = q.reshape(bsz, L, B_HEADS, B_HEAD_DIM)
    c_kv = rms_norm(c_kv, kv_norm_g)
    q_abs = jnp.einsum('blhd,chd->blhc', q, w_uk) * (B_HEAD_DIM ** -0.5)
    q_i = q_i.reshape(bsz, L, B_IDX_HEADS, B_IDX_DIM)
    k_i = rms_norm(k_i, kidx_g)
    w_i = w_i * (B_IDX_HEADS ** -0.5 * B_IDX_DIM ** -0.5)
    k_top = min(B_TOPK_MAX, L // 4)
    nb = L // B_QBLK
    pos_all = jnp.arange(L, dtype=jnp.int32)

    def to_blocks(a):
        return a.reshape(bsz, nb, B_QBLK, *a.shape[2:]).swapaxes(0, 1)

    def block(args):
        qa, qi, wi, start = args
        t = start + jnp.arange(B_QBLK, dtype=jnp.int32)
        sc = jnp.einsum('bthd,bsd->bths', qi, k_i)
        sc = jnp.einsum('bths,bth->bts', jax.nn.relu(sc), wi).astype(jnp.float32)
        causal = pos_all[None, :] <= t[:, None]
        sc = jnp.where(causal[None], sc, -jnp.inf)
        _, sel = lax.top_k(sc, k_top)
        kv_sel = jax.vmap(lambda cb, ib: cb[ib])(c_kv, sel)
        dist = t[None, :, None] - sel
        valid = dist >= 0
        bias = rel_bias[t5_bucket(dist)]
        logits = (jnp.einsum('bthc,btkc->bthk', qa, kv_sel).astype(jnp.float32)
                  + jnp.transpose(bias, (0, 1, 3, 2)).astype(jnp.float32))
        logits = jnp.where(valid[:, :, None, :], logits, -jnp.inf)
        p = jax.nn.softmax(logits, axis=-1).astype(qa.dtype)
        o_lat = jnp.einsum('bthk,btkc->bthc', p, kv_sel)
        o = jnp.einsum('bthc,chd->bthd', o_lat, w_uv)
        return o.reshape(bsz, B_QBLK, B_HEADS * B_V_DIM)

    starts = jnp.arange(nb, dtype=jnp.int32) * B_QBLK
    o = lax.map(block, (to_blocks(q_abs), to_blocks(q_i), to_blocks(w_i), starts))
    o = o.swapaxes(0, 1).reshape(bsz, L, B_HEADS * B_V_DIM)
    return o @ w_out


def grouped_moe(h, router_w, router_b, w_gate, w_up, w_down):
    bsz, L, D = h.shape
    N = bsz * L
    hf = h.reshape(N, D)
    aff = jax.nn.sigmoid((hf @ router_w).astype(jnp.float32))
    sel = aff + router_b.astype(jnp.float32)
    sel_g = sel.reshape(N, N_GROUPS, EXPERTS_PER_GROUP)
    grp_score = lax.top_k(sel_g, 2)[0].sum(-1)
    grp = jnp.argmax(grp_score, axis=-1).astype(jnp.int32)
    in_grp = jnp.take_along_axis(sel_g, grp[:, None, None], axis=1)[:, 0]
    _, local = lax.top_k(in_grp, TOP_K)
    experts = grp[:, None] * EXPERTS_PER_GROUP + local
    gates = jnp.take_along_axis(aff, experts, axis=1)
    gates = gates / jnp.sum(gates, axis=-1, keepdims=True)

    A = N * TOP_K
    e_flat = experts.reshape(A)
    order = jnp.argsort(e_flat)
    e_sorted = e_flat[order]
    counts = jnp.bincount(e_flat, length=N_EXPERTS)
    padded = ((counts + MOE_BLOCK - 1) // MOE_BLOCK) * MOE_BLOCK
    pad_end = jnp.cumsum(padded)
    pad_start = pad_end - padded
    seg_start = jnp.cumsum(counts) - counts
    rank = jnp.arange(A, dtype=jnp.int32) - seg_start[e_sorted]
    slot_sorted = (pad_start[e_sorted] + rank).astype(jnp.int32)
    P = A + N_EXPERTS * MOE_BLOCK
    n_blk = P // MOE_BLOCK
    tok_of_slot = jnp.zeros((P,), jnp.int32).at[slot_sorted].set((order // TOP_K).astype(jnp.int32))
    blk_start = jnp.arange(n_blk, dtype=jnp.int32) * MOE_BLOCK
    blk_expert = jnp.minimum(jnp.searchsorted(pad_end, blk_start, side='right'),
                             N_EXPERTS - 1).astype(jnp.int32)
    x_blk = hf[tok_of_slot].reshape(n_blk, MOE_BLOCK, D)

    def expert_block(args):
        xb, e = args
        return (jax.nn.silu(xb @ w_gate[e]) * (xb @ w_up[e])) @ w_down[e]

    y_slot = lax.map(expert_block, (x_blk, blk_expert)).reshape(P, D)
    slot_of_assign = jnp.zeros((A,), jnp.int32).at[order].set(slot_sorted)
    y = y_slot[slot_of_assign].reshape(N, TOP_K, D)
    out = jnp.einsum('nk,nkd->nd', gates.astype(y.dtype), y)
    return out.reshape(bsz, L, D)


def setup_inputs(seed: int = 0) -> dict:
    key = jax.random.key(seed)
    ks = jax.random.split(key, 32)
    f32 = jnp.float32
    D = D_MODEL

    def nrm(k, shape, scale):
        return jax.random.normal(k, shape, f32) * scale

    return {
        "x": nrm(ks[0], (BATCH, SEQ, D), 1.0),
        "c": nrm(ks[1], (BATCH, D), 1.0),
        "ada_w": nrm(ks[2], (DEPTH, D, 6 * D), 0.5 * D ** -0.5),
        "ada_b": nrm(ks[3], (DEPTH, 6 * D), 0.02),
        "norm1_g": 1.0 + nrm(ks[4], (DEPTH, D), 0.05),
        "norm2_g": 1.0 + nrm(ks[5], (DEPTH, D), 0.05),
        "a_w_in": nrm(ks[6], (N_A_LAYERS, D, 2 * A_INNER), D ** -0.5),
        "a_ln_g": 1.0 + nrm(ks[7], (N_A_LAYERS, A_INNER), 0.05),
        "a_ln_b": nrm(ks[8], (N_A_LAYERS, A_INNER), 0.02),
        "a_w_sp": nrm(ks[9], (N_A_LAYERS, A_GROUPS, A_CHUNK, A_CHUNK), A_CHUNK ** -0.5),
        "a_b_sp": 1.0 + nrm(ks[10], (N_A_LAYERS, A_GROUPS, A_CHUNK), 0.1),
        "a_w_out": nrm(ks[11], (N_A_LAYERS, A_INNER, D), A_INNER ** -0.5),
        "b_w_in": nrm(ks[12], (N_B_LAYERS, D, B_PROJ), D ** -0.5),
        "b_kv_norm_g": 1.0 + nrm(ks[13], (N_B_LAYERS, B_KV_LATENT), 0.05),
        "b_w_uk": nrm(ks[14], (N_B_LAYERS, B_KV_LATENT, B_HEADS, B_HEAD_DIM), B_KV_LATENT ** -0.5),
        "b_w_uv": nrm(ks[15], (N_B_LAYERS, B_KV_LATENT, B_HEADS, B_V_DIM), B_KV_LATENT ** -0.5),
        "b_kidx_g": 1.0 + nrm(ks[16], (N_B_LAYERS, B_IDX_DIM), 0.05),
        "b_w_out": nrm(ks[17], (N_B_LAYERS, B_HEADS * B_V_DIM, D), (B_HEADS * B_V_DIM) ** -0.5),
        "rel_bias": nrm(ks[18], (REL_BUCKETS, B_HEADS), 0.3),
        "router_w": nrm(ks[19], (D, N_EXPERTS), D ** -0.5),
        "router_b": nrm(ks[20], (N_EXPERTS,), 0.01),
        "moe_w_gate": nrm(ks[21], (DEPTH, N_EXPERTS, D, D_EXPERT), D ** -0.5),
        "moe_w_up": nrm(ks[22], (DEPTH, N_EXPERTS, D, D_EXPERT), D ** -0.5),
        "moe_w_down": nrm(ks[23], (DEPTH, N_EXPERTS, D_EXPERT, D), D_EXPERT ** -0.5),
        "final_g": 1.0 + nrm(ks[24], (D,), 0.05),
    }


def reference(x, c, ada_w, ada_b, norm1_g, norm2_g, a_w_in, a_ln_g, a_ln_b, a_w_sp, a_b_sp,
              a_w_out, b_w_in, b_kv_norm_g, b_w_uk, b_w_uv, b_kidx_g, b_w_out, rel_bias,
              router_w, router_b, moe_w_gate, moe_w_up, moe_w_down, final_g):
    mod = jnp.einsum('bd,lde->lbe', jax.nn.silu(c), ada_w) + ada_b[:, None, :]
    h = x
    for i in range(DEPTH):
        sh1, sc1, g1, sh2, sc2, g2 = jnp.split(mod[i], 6, axis=-1)
        hn = rms_norm(h, norm1_g[i]) * (1.0 + sc1[:, None, :]) + sh1[:, None, :]
        j = i // N_MIXERS
        if i % N_MIXERS == 0:
            y = gmlp_mixer(hn, a_w_in[j], a_ln_g[j], a_ln_b[j], a_w_sp[j], a_b_sp[j], a_w_out[j])
        else:
            y = dsa_mixer(hn, b_w_in[j], b_kv_norm_g[j], b_w_uk[j], b_w_uv[j], b_kidx_g[j],
                          b_w_out[j], rel_bias)
        h = h + g1[:, None, :] * y
        hn = rms_norm(h, norm2_g[i]) * (1.0 + sc2[:, None, :]) + sh2[:, None, :]
        h = h + g2[:, None, :] * grouped_moe(hn, router_w, router_b, moe_w_gate[i],
                                             moe_w_up[i], moe_w_down[i])
    return rms_norm(h, final_g)
```

```python
import numpy as np
from contextlib import ExitStack
import concourse.bass as bass
import concourse.mybir as mybir
from concourse.bass_utils import run_bass_kernel_spmd

F32 = mybir.dt.float32
BF16 = mybir.dt.bfloat16
AF = mybir.ActivationFunctionType
ALU = mybir.AluOpType
AX = mybir.AxisListType

D = 1024
KC = 8
NT = 2048
NTILE = 16
SEQ = 4096
DE = 3584
NE = 16
EPS = 1e-6
ENGS = ["pe", "dve", "act", "pool", "sp"]
BIG = 1.0e30


def my_qblocks(par):
    offs = (0, 3) if par == 0 else (1, 2)
    return [4 * (i // 2) + offs[i % 2] for i in range(16)]


class Buf:
    __slots__ = ("name", "writer", "readers")

    def __init__(self, name):
        self.name = name
        self.writer = None
        self.readers = []


class _Rec:
    def __getattr__(self, name):
        def f(*a, **k):
            self.call = (name, a, k)
            return self
        return f


class Sched:
    def __init__(self, nc, same_engine_sync=True):
        self.nc = nc
        self.prog = {e: [] for e in ENGS}
        self.cnt = {e: 0 for e in ENGS}
        self.sems = {}
        self.waited = {e: {} for e in ENGS}
        self.same = same_engine_sync
        self._stack = []
        self.dma_sems = {}
        self.dma_cnt = {}
        self.bufs_ = {}
        for e in ENGS:
            self.sems[e] = self._sem("s_" + e)

    def _sem(self, name):
        g = self.nc.semaphore(name)
        s = g.__enter__()
        self._stack.append(g)
        return s

    def B(self, *key):
        b = self.bufs_.get(key)
        if b is None:
            b = Buf(str(key))
            self.bufs_[key] = b
        return b

    def _wait(self, eng, dep):
        key, val = dep[1], dep[2]
        if dep[0] == "eng" and key == eng:
            if not self.same or val > self.cnt[eng]:
                return
        if self.waited[eng].get((dep[0], key), 0) >= val:
            return
        self.waited[eng][(dep[0], key)] = val
        sem = self.sems[key] if dep[0] == "eng" else self.dma_sems[key]
        self.prog[eng].append(lambda e, sem=sem, val=val: e.wait_ge(sem, val))

    def _deps(self, eng, reads, writes):
        for b in reads:
            if b.writer is not None:
                self._wait(eng, b.writer)
        for b in writes:
            if b.writer is not None:
                self._wait(eng, b.writer)
            for r in b.readers:
                self._wait(eng, r)

    def _mark(self, tok, reads, writes):
        for b in reads:
            b.readers.append(tok)
            if len(b.readers) > 64:
                b.readers = b.readers[-48:]
        for b in writes:
            b.writer = tok
            b.readers = []

    def op(self, eng, fn, reads=(), writes=(), inc=True):
        self._deps(eng, reads, writes)
        rec = _Rec()
        fn(rec)
        name, a, k = rec.call
        if inc:
            self.cnt[eng] += 1
            sem = self.sems[eng]
            self.prog[eng].append(lambda e, name=name, a=a, k=k, sem=sem: getattr(e, name)(*a, **k).then_inc(sem, 1))
            tok = ("eng", eng, self.cnt[eng])
        else:
            self.prog[eng].append(lambda e, name=name, a=a, k=k: getattr(e, name)(*a, **k))
            tok = ("eng", eng, self.cnt[eng] + 1)
        self._mark(tok, reads, writes)

    def dma(self, eng, fn, reads=(), writes=(), stream="d", incv=16):
        if stream not in self.dma_sems:
            self.dma_sems[stream] = self._sem("dq_" + stream)
            self.dma_cnt[stream] = 0
        self._deps(eng, reads, writes)
        self.dma_cnt[stream] += incv
        sem = self.dma_sems[stream]
        rec = _Rec()
        fn(rec)
        name, a, k = rec.call
        self.prog[eng].append(lambda e, name=name, a=a, k=k, sem=sem, incv=incv: getattr(e, name)(*a, **k).then_inc(sem, incv))
        tok = ("dma", stream, self.dma_cnt[stream])
        self._mark(tok, reads, writes)
        return tok

    def barrier(self):
        for e in ENGS:
            for o in ENGS:
                if o != e and self.cnt[o] > 0:
                    self._wait(e, ("eng", o, self.cnt[o]))
            for s, v in self.dma_cnt.items():
                self._wait(e, ("dma", s, v))

    def finish(self):
        nc = self.nc
        with nc.Block() as block:
            def mk(name):
                def body(e):
                    for f in self.prog[name]:
                        f(e)
                return body
            block.tensor(mk("pe"))
            block.vector(mk("dve"))
            block.scalar(mk("act"))
            block.gpsimd(mk("pool"))
            block.sync(mk("sp"))
        for g in reversed(self._stack):
            g.__exit__(None, None, None)


class Ctx:
    pass


def dram_in(nc, name, shape, dt=F32):
    return nc.dram_tensor(name, list(shape), dt, kind="ExternalInput").ap()


def setup_common(C, es):
    nc, S = C.nc, C.S
    sb = lambda n, s, d: es.enter_context(nc.sbuf_tensor(n, s, d))
    C.ident32 = sb("ident32", [128, 128], F32)
    C.identb = sb("identb", [128, 128], BF16)
    C.ones32 = sb("ones32", [128, 128], F32)
    C.onesb = sb("onesb", [128, 128], BF16)
    C.psb = [es.enter_context(nc.psum_tensor(f"psb{i}", [128, 512], F32)) for i in range(7)]
    C.pst = es.enter_context(nc.psum_tensor("pst", [128, 1024], BF16))
    C.pB = [S.B("ps", i) for i in range(7)]
    C.ptB = S.B("pst")
    cb = S.B("consts")
    S.op("pool", lambda e: e.memset(C.ident32[:], 1.0), writes=[cb])
    S.op("pool", lambda e: e.affine_select(out=C.ident32[:], in_=C.ident32[:], pattern=[[-1, 128]],
                                           compare_op=ALU.is_equal, fill=0.0, base=0, channel_multiplier=1),
         reads=[cb], writes=[cb])
    S.op("pool", lambda e: e.tensor_copy(out=C.identb[:], in_=C.ident32[:]), reads=[cb], writes=[cb])
    S.op("pool", lambda e: e.memset(C.ones32[:], 1.0), writes=[cb])
    S.op("pool", lambda e: e.memset(C.onesb[:], 1.0), writes=[cb])
    C.epsc = sb("epsc", [128, 1], F32)
    S.op("pool", lambda e: e.memset(C.epsc[:], EPS), writes=[cb])
    C.cb = cb
    C.psrot = 0


def hB(C, k, t0, t1):
    return [C.S.B("h", k, t) for t in range(t0 // 128, (t1 + 127) // 128)]


def alloc_h(C, es, tag=""):
    C.h = es.enter_context(C.nc.sbuf_tensor("h" + tag, [128, KC, NT], F32))


def load_h(C, src):
    S = C.S
    v = src.rearrange("(k p) t -> p k t", p=128)
    for k in range(KC):
        S.dma("sp", lambda e, k=k: e.dma_start(out=C.h[:, k, :], in_=v[:, k, :]),
              writes=hB(C, k, 0, NT), stream=f"ldh{k}")


def prologue_mod(C, es, cT, ada_w, ada_bT, n1g, n2g, tag):
    nc, S = C.nc, C.S
    sb = lambda n, s, d: es.enter_context(nc.sbuf_tensor(n + tag, s, d))
    m = Ctx()
    m.mod = sb("mod", [128, 48], F32)
    m.gmod1 = sb("gmod1", [128, KC], F32)
    m.gmod2 = sb("gmod2", [128, KC], F32)
    mB = S.B("mod" + tag)
    with ExitStack() as tmp:
        tb = lambda n, s, d: tmp.enter_context(nc.sbuf_tensor(n + tag, s, d))
        ct = tb("ct", [128, KC], F32)
        sc2 = tb("sc2", [128, KC, 2], F32)
        adab = tb("adab", [128, 48], F32)
        g1t = tb("g1t", [128, KC], F32)
        g2t = tb("g2t", [128, KC], F32)
        slabs = [tb(f"adaw{i}", [128, KC, 512], F32) for i in range(2)]
        tB = S.B("protmp" + tag)
        S.dma("sp", lambda e: e.dma_start(out=ct[:], in_=cT), writes=[tB], stream="pro0")
        S.dma("sp", lambda e: e.dma_start(out=adab[:], in_=ada_bT), writes=[tB], stream="pro0")
        S.dma("sp", lambda e: e.dma_start(out=g1t[:], in_=n1g), writes=[tB], stream="pro0")
        S.dma("sp", lambda e: e.dma_start(out=g2t[:], in_=n2g), writes=[tB], stream="pro0")
        scB = S.B("sc2" + tag)
        for j in range(2):
            S.op("act", lambda e, j=j: e.activation(out=sc2[:, :, j], in_=ct[:], func=AF.Silu), reads=[tB], writes=[scB])
        wv = ada_w.rearrange("(k p) n -> p k n", p=128)
        pb = C.psb[6]
        pB = C.pB[6]
        for s in range(12):
            sl = slabs[s % 2]
            slB = S.B("adaw" + tag, s % 2)
            S.dma("sp", lambda e, s=s, sl=sl: e.dma_start(out=sl[:], in_=wv[:, :, s * 512:(s + 1) * 512]),
                  writes=[slB], stream=f"adaw{s % 2}")
            for jj in range(4):
                j = 4 * s + jj
                for k in range(KC):
                    S.op("pe", lambda e, j=j, jj=jj, k=k, sl=sl: e.matmul(
                        pb[:, 2 * j:2 * j + 2], lhsT=sl[:, k, jj * 128:(jj + 1) * 128], rhs=sc2[:, k, :],
                        start=(k == 0), stop=(k == KC - 1)),
                        reads=[slB, scB], writes=[pB], inc=(k == KC - 1))
        pv = pb[:, 0:96].rearrange("p (j two) -> p j two", two=2)
        S.op("dve", lambda e: e.tensor_tensor(out=m.mod[:], in0=pv[:, :, 0], in1=adab[:], op=ALU.add),
             reads=[pB, tB], writes=[mB])
        for (gm, gt, part) in ((m.gmod1, g1t, 1), (m.gmod2, g2t, 4)):
            S.op("dve", lambda e, gm=gm, part=part: e.tensor_scalar(
                out=gm[:], in0=m.mod[:, part * 8:(part + 1) * 8], scalar1=1.0, scalar2=1.0, op0=ALU.add, op1=ALU.mult),
                reads=[mB], writes=[mB])
            S.op("dve", lambda e, gm=gm, gt=gt: e.tensor_tensor(out=gm[:], in0=gm[:], in1=gt[:], op=ALU.mult),
                 reads=[mB, tB], writes=[mB])
        S.barrier()
    m.B = mB
    return m


def rms_mod(C, t0, W, gmod, sh_col, mB, sq32, sqB, rstd, rsB, out_bf, outB_fn, out32=None, o32B=None, src=None, srcB_fn=None):
    S = C.S
    src = C.h if src is None else src
    srcB = (lambda k: hB(C, k, t0, t0 + W)) if srcB_fn is None else srcB_fn
    pb, pB = C.psb[6], C.pB[6]
    for k in range(KC):
        S.op("act", lambda e, k=k: e.activation(out=sq32[:, k, 0:W], in_=src[:, k, t0:t0 + W], func=AF.Square),
             reads=srcB(k), writes=[sqB])
    for k in range(KC):
        S.op("pe", lambda e, k=k: e.matmul(pb[:, 0:W], lhsT=C.ones32[:], rhs=sq32[:, k, 0:W], start=(k == 0), stop=(k == KC - 1)),
             reads=[sqB, C.cb], writes=[pB], inc=(k == KC - 1))
    S.op("act", lambda e: e.activation(out=rstd[:, 0:W], in_=pb[:, 0:W], func=AF.Sqrt, bias=C.epsc[:, 0:1], scale=1.0 / 1024.0),
         reads=[pB, C.cb], writes=[rsB])
    S.op("dve", lambda e: e.reciprocal(out=rstd[:, 0:W], in_=rstd[:, 0:W]), reads=[rsB], writes=[rsB])
    for k in range(KC):
        S.op("dve", lambda e, k=k: e.scalar_tensor_tensor(out=sq32[:, k, 0:W], in0=src[:, k, t0:t0 + W], scalar=gmod[:, k:k + 1],
                                                          in1=rstd[:, 0:W], op0=ALU.mult, op1=ALU.mult),
             reads=srcB(k) + [mB, rsB], writes=[sqB])
        S.op("act", lambda e, k=k: e.activation(out=out_bf(k), in_=sq32[:, k, 0:W], func=AF.Identity,
                                                bias=sh_col(k), scale=1.0),
             reads=[sqB, mB], writes=outB_fn(k))
        if out32 is not None:
            S.op("act", lambda e, k=k: e.activation(out=out32[:, k, 0:W], in_=sq32[:, k, 0:W], func=AF.Identity,
                                                    bias=sh_col(k), scale=1.0),
                 reads=[sqB, mB], writes=[o32B])


def moe_layer(C, m, router_w, router_bb, wgu_sh, wd_sh, tag):
    nc, S = C.nc, C.S
    REPL = isinstance(wgu_sh, tuple)
    if REPL:
        wg_full, wu_full, wd_full = wgu_sh
        return _moe_body(C, m, router_w, router_bb, tag, None, None, None, (wg_full, wu_full, wd_full))
    EG = 4
    NGRP = NE // EG
    bnc_gu = [nc.dram_tensor(f"bnc_gu{tag}_{i}", [EG * 256, DE], F32).ap() for i in range(NGRP)]
    bnc_d = [nc.dram_tensor(f"bnc_d{tag}_{i}", [EG * 448, D], F32).ap() for i in range(NGRP)]
    gat_gu = [nc.dram_tensor(f"gat_gu{tag}_{i}", [8 * EG * 256, DE], F32).ap() for i in range(NGRP)]
    gat_d = [nc.dram_tensor(f"gat_d{tag}_{i}", [8 * EG * 448, D], F32).ap() for i in range(NGRP)]
    wgu2 = wgu_sh.rearrange("e r f -> (e r) f")
    wd2 = wd_sh.rearrange("e r d -> (e r) d")
    for i in range(NGRP):
        bB, gB = S.B("bnc" + tag, i), S.B("gat" + tag, i)
        S.dma("sp", lambda e: e.dma_start(out=bnc_gu[i], in_=wgu2[i * EG * 256:(i + 1) * EG * 256, :]), writes=[bB], stream=f"bnc{tag}{i}")
        S.dma("sp", lambda e: e.dma_start(out=bnc_d[i], in_=wd2[i * EG * 448:(i + 1) * EG * 448, :]), writes=[bB], stream=f"bnc{tag}{i}")
    for i in range(NGRP):
        bB, gB = S.B("bnc" + tag, i), S.B("gat" + tag, i)
        S.dma("pool", lambda e: e.collective_compute("AllGather", ALU.bypass, replica_groups=[list(range(8))],
                                                     ins=[bnc_gu[i].opt()], outs=[gat_gu[i].opt()]),
              reads=[bB], writes=[gB], stream=f"cc{tag}{i}", incv=1)
        S.dma("pool", lambda e: e.collective_compute("AllGather", ALU.bypass, replica_groups=[list(range(8))],
                                                     ins=[bnc_d[i].opt()], outs=[gat_d[i].opt()]),
              reads=[bB], writes=[gB], stream=f"cc{tag}{i}", incv=1)
    return _moe_body(C, m, router_w, router_bb, tag, EG, gat_gu, gat_d, None)


def _moe_body(C, m, router_w, router_bb, tag, EG, gat_gu, gat_d, full):
    nc, S = C.nc, C.S
    with ExitStack() as es:
        sb = lambda n, s, d: es.enter_context(nc.sbuf_tensor(n + tag, s, d))
        hn = sb("hn", [128, KC, NT], BF16)
        sq32 = sb("msq32", [128, KC, 256], F32)
        hn32 = sb("mhn32", [128, KC, 256], F32)
        rstd = sb("mrstd", [128, 256], F32)
        rw = sb("rw", [128, KC, NE], F32)
        rbb = sb("rbb", [128, NE], F32)
        gT = sb("gT", [16, NT], F32)
        selm = sb("selm", [16, NE, 128], F32)
        rt = sb("rt", [128, 160], F32)
        pad = sb("rpad", [128, 4, 8], F32)
        top8 = sb("rtop8", [128, 4, 8], F32)
        SL = 512
        NSL = DE // SL
        FC = SL // 128
        wgs = [sb(f"wgs{i}", [128, KC, SL], BF16) for i in range(2)]
        wus = [sb(f"wus{i}", [128, KC, SL], BF16) for i in range(2)]
        wds = [sb(f"wds{i}", [128, FC, D], BF16) for i in range(2)]
        acts = [sb(f"act{i}", [128, FC, 512], BF16) for i in range(2)]
        gbc = [sb(f"gbc{i}", [128, NT], BF16) for i in range(2)]
        sg = [sb(f"sg{i}", [128, 512], F32) for i in range(2)]
        hnB = lambda k, t0, t1: [S.B("hn" + tag, k, t) for t in range(t0 // 128, (t1 + 127) // 128)]
        sqB, rsB, o32B = S.B("msq" + tag), S.B("mrs" + tag), S.B("mh32" + tag)
        cB = S.B("mconst" + tag)
        S.dma("sp", lambda e: e.dma_start(out=rw[:], in_=router_w.rearrange("(k p) n -> p k n", p=128)), writes=[cB], stream="mc")
        S.dma("sp", lambda e: e.dma_start(out=rbb[:], in_=router_bb), writes=[cB], stream="mc")
        S.op("pool", lambda e: e.memset(selm[:], 1.0), writes=[cB])
        S.op("pool", lambda e: e.affine_select(out=selm[:], in_=selm[:], pattern=[[-1, NE], [0, 128]], compare_op=ALU.is_equal,
                                               fill=0.0, base=0, channel_multiplier=1), reads=[cB], writes=[cB])
        S.op("pool", lambda e: e.memset(pad[:], -BIG), writes=[cB])
        gTB = S.B("gT" + tag)
        rtB = S.B("rt" + tag)

        import os
        NE_RUN = int(os.environ.get("MOE_NE", str(NE)))
        units = [(e_, s_) for e_ in range(NE_RUN) for s_ in range(NSL)]
        if full is None:
            guv = [g.rearrange("(r el t p) f -> el t p r f", r=8, el=EG, t=2, p=128) for g in gat_gu]
        else:
            fwg = full[0].rearrange("e (k p) f -> e p k f", p=128)
            fwu = full[1].rearrange("e (k p) f -> e p k f", p=128)
            fwd = full[2].rearrange("e (c p) d -> e p c d", p=128)

        def issue_w(ui):
            e_, s_ = units[ui]
            sl = ui % 2
            if full is not None:
                wB = S.B("mw" + tag, sl)
                S.dma("pool", lambda e: e.dma_start(out=wgs[sl][:], in_=fwg[e_, :, :, s_ * SL:(s_ + 1) * SL]), writes=[wB], stream=f"mw{sl}")
                S.dma("pool", lambda e: e.dma_start(out=wus[sl][:], in_=fwu[e_, :, :, s_ * SL:(s_ + 1) * SL]), writes=[wB], stream=f"mw{sl}")
                S.dma("pool", lambda e: e.dma_start(out=wds[sl][:], in_=fwd[e_, :, s_ * FC:(s_ + 1) * FC, :]), writes=[wB], stream=f"mw{sl}")
                return
            gi, el = e_ // EG, e_ % EG
            wB = S.B("mw" + tag, sl)
            gB = S.B("gat" + tag, gi)
            S.dma("pool", lambda e: e.dma_start(out=wgs[sl][:], in_=guv[gi][el, 0][:, :, s_ * SL:(s_ + 1) * SL]), reads=[gB], writes=[wB], stream=f"mw{sl}")
            S.dma("pool", lambda e: e.dma_start(out=wus[sl][:], in_=guv[gi][el, 1][:, :, s_ * SL:(s_ + 1) * SL]), reads=[gB], writes=[wB], stream=f"mw{sl}")
            for fc in range(FC):
                f0 = s_ * SL + fc * 128
                f = f0
                while f < f0 + 128:
                    r, i0 = divmod(f, 448)
                    n = min(f0 + 128 - f, 448 - i0)
                    row = r * EG * 448 + el * 448 + i0
                    p0 = f - f0
                    S.dma("pool", lambda e: e.dma_start(out=wds[sl][p0:p0 + n, fc, :], in_=gat_d[gi][row:row + n, :]), reads=[gB], writes=[wB], stream=f"mw{sl}")
                    f += n

        issue_w(0)
        issue_w(1)

        sh_col = lambda k: m.mod[:, 24 + k:24 + k + 1]
        for t0 in range(0, NT, 256):
            rms_mod(C, t0, 256, m.gmod2, sh_col, m.B, sq32, sqB, rstd, rsB,
                    out_bf=lambda k, t0=t0: hn[:, k, t0:t0 + 256], outB_fn=lambda k, t0=t0: hnB(k, t0, t0 + 256),
                    out32=hn32, o32B=o32B)
            for tl in range(2):
                ti = t0 // 128 + tl
                pb, pB = C.psb[5], C.pB[5]
                for k in range(KC):
                    S.op("pe", lambda e, k=k, tl=tl: e.matmul(pb[:, 0:NE], lhsT=hn32[:, k, tl * 128:(tl + 1) * 128], rhs=rw[:, k, :],
                                                              start=(k == 0), stop=(k == KC - 1)),
                         reads=[o32B, cB], writes=[pB], inc=(k == KC - 1))
                aff = rt[:, 0:16]
                sel = rt[:, 16:32]
                gsc = rt[:, 32:36]
                gmax = rt[:, 36:37]
                gsel = rt[:, 40:44]
                tmp4 = rt[:, 44:48]
                thr2 = rt[:, 48:49]
                M = rt[:, 64:80]
                ga = rt[:, 80:96]
                den = rt[:, 96:97]
                gates = rt[:, 112:128]
                S.op("act", lambda e: e.activation(out=aff, in_=pb[:, 0:NE], func=AF.Sigmoid), reads=[pB], writes=[rtB])
                S.op("dve", lambda e: e.tensor_tensor(out=sel, in0=aff, in1=rbb[:], op=ALU.add), reads=[rtB, cB], writes=[rtB])
                S.op("dve", lambda e: e.tensor_copy(out=pad[:, :, 0:4], in_=sel.rearrange("p (g j) -> p g j", j=4)), reads=[rtB, cB], writes=[rtB])
                for g in range(4):
                    S.op("dve", lambda e, g=g: e.max(out=top8[:, g, :], in_=pad[:, g, :]), reads=[rtB], writes=[rtB])
                S.op("dve", lambda e: e.tensor_tensor(out=gsc, in0=top8[:, :, 0], in1=top8[:, :, 1], op=ALU.add), reads=[rtB], writes=[rtB])
                S.op("dve", lambda e: e.tensor_reduce(out=gmax, in_=gsc, axis=AX.X, op=ALU.max), reads=[rtB], writes=[rtB])
                S.op("dve", lambda e: e.tensor_scalar(out=gsel, in0=gsc, scalar1=gmax, scalar2=None, op0=ALU.is_ge), reads=[rtB], writes=[rtB])
                acc = rt[:, 100:101]
                nacc = rt[:, 101:102]
                S.op("dve", lambda e: e.tensor_copy(out=acc, in_=gsel[:, 0:1]), reads=[rtB], writes=[rtB])
                for g in range(1, 4):
                    S.op("dve", lambda e: e.tensor_scalar(out=nacc, in0=acc, scalar1=-1.0, scalar2=1.0, op0=ALU.mult, op1=ALU.add), reads=[rtB], writes=[rtB])
                    S.op("dve", lambda e, g=g: e.tensor_tensor(out=gsel[:, g:g + 1], in0=gsel[:, g:g + 1], in1=nacc, op=ALU.mult), reads=[rtB], writes=[rtB])
                    if g < 3:
                        S.op("dve", lambda e, g=g: e.tensor_tensor(out=acc, in0=acc, in1=gsel[:, g:g + 1], op=ALU.add), reads=[rtB], writes=[rtB])
                S.op("dve", lambda e: e.tensor_tensor(out=tmp4, in0=gsel, in1=top8[:, :, 1], op=ALU.mult), reads=[rtB], writes=[rtB])
                S.op("dve", lambda e: e.tensor_reduce(out=thr2, in_=tmp4, axis=AX.X, op=ALU.add), reads=[rtB], writes=[rtB])
                for g in range(4):
                    S.op("dve", lambda e, g=g: e.tensor_scalar(out=M[:, 4 * g:4 * g + 4], in0=sel[:, 4 * g:4 * g + 4], scalar1=thr2,
                                                               scalar2=gsel[:, g:g + 1], op0=ALU.is_ge, op1=ALU.mult),
                         reads=[rtB], writes=[rtB])
                S.op("dve", lambda e: e.tensor_tensor(out=ga, in0=aff, in1=M, op=ALU.mult), reads=[rtB], writes=[rtB])
                S.op("dve", lambda e: e.tensor_reduce(out=den, in_=ga, axis=AX.X, op=ALU.add), reads=[rtB], writes=[rtB])
                S.op("dve", lambda e: e.tensor_scalar_max(out=den, in0=den, scalar1=1e-20), reads=[rtB], writes=[rtB])
                S.op("dve", lambda e: e.reciprocal(out=den, in_=den), reads=[rtB], writes=[rtB])
                S.op("dve", lambda e: e.tensor_scalar(out=gates, in0=ga, scalar1=den, scalar2=None, op0=ALU.mult), reads=[rtB], writes=[rtB])
                S.op("pe", lambda e: e.transpose(out=pb[0:16, 128:256], in_=gates, identity=C.ident32[:]), reads=[rtB, C.cb], writes=[pB])
                S.op("act", lambda e, ti=ti: e.activation(out=gT[:, ti * 128:(ti + 1) * 128], in_=pb[0:16, 128:256], func=AF.Copy),
                     reads=[pB], writes=[gTB])

        rot = 0
        for ui, (e_, s_) in enumerate(units):
            sl = ui % 2
            wB = S.B("mw" + tag, sl)
            if s_ == 0:
                gb = gbc[e_ % 2]
                gbB = S.B("gbc" + tag, e_ % 2)
                for g in range(4):
                    pb, pB = C.psb[5], C.pB[5]
                    S.op("pe", lambda e, g=g, e_=e_: e.matmul(pb[:, :], lhsT=selm[:, e_, :], rhs=gT[:, g * 512:(g + 1) * 512], start=True, stop=True),
                         reads=[cB, gTB], writes=[pB])
                    S.op("act", lambda e, g=g, gb=gb: e.activation(out=gb[:, g * 512:(g + 1) * 512], in_=pb[:, :], func=AF.Copy),
                         reads=[pB], writes=[gbB])
            gb = gbc[e_ % 2]
            gbB = S.B("gbc" + tag, e_ % 2)
            for g in range(4):
                tc0 = g * 512
                act = acts[rot % 2]
                aB = S.B("act" + tag, rot % 2)
                rot += 1
                for fc in range(FC):
                    pg, pgB = C.psb[fc % 2], C.pB[fc % 2]
                    pu, puB = C.psb[2 + fc % 2], C.pB[2 + fc % 2]
                    for k in range(KC):
                        S.op("pe", lambda e, k=k, fc=fc, pg=pg: e.matmul(pg[:, :], lhsT=wgs[sl][:, k, fc * 128:(fc + 1) * 128], rhs=hn[:, k, tc0:tc0 + 512],
                                                                       start=(k == 0), stop=(k == KC - 1)),
                             reads=[wB] + hnB(k, tc0, tc0 + 512), writes=[pgB], inc=(k == KC - 1))
                    for k in range(KC):
                        S.op("pe", lambda e, k=k, fc=fc, pu=pu: e.matmul(pu[:, :], lhsT=wus[sl][:, k, fc * 128:(fc + 1) * 128], rhs=hn[:, k, tc0:tc0 + 512],
                                                                       start=(k == 0), stop=(k == KC - 1)),
                             reads=[wB] + hnB(k, tc0, tc0 + 512), writes=[puB], inc=(k == KC - 1))
                    sgt = sg[fc % 2]
                    sgB = S.B("sg" + tag, fc % 2)
                    S.op("act", lambda e, pg=pg, sgt=sgt: e.activation(out=sgt[:], in_=pg[:, :], func=AF.Silu), reads=[pgB], writes=[sgB])
                    S.op("dve", lambda e, pu=pu, sgt=sgt: e.tensor_tensor(out=sgt[:], in0=sgt[:], in1=pu[:, :], op=ALU.mult), reads=[sgB, puB], writes=[sgB])
                    S.op("dve", lambda e, fc=fc, act=act, sgt=sgt, gb=gb: e.tensor_tensor(out=act[:, fc, :], in0=sgt[:], in1=gb[:, tc0:tc0 + 512], op=ALU.mult),
                         reads=[sgB, gbB], writes=[aB])
                for dk in range(KC):
                    py, pyB = C.psb[4 + dk % 2], C.pB[4 + dk % 2]
                    for fc in range(FC):
                        S.op("pe", lambda e, fc=fc, dk=dk, py=py, act=act: e.matmul(py[:, :], lhsT=wds[sl][:, fc, dk * 128:(dk + 1) * 128], rhs=act[:, fc, :],
                                                                                start=(fc == 0), stop=(fc == FC - 1)),
                             reads=[wB, aB], writes=[pyB], inc=(fc == FC - 1))
                    S.op("dve", lambda e, dk=dk, py=py: e.scalar_tensor_tensor(out=C.h[:, dk, tc0:tc0 + 512], in0=py[:, :], scalar=m.mod[:, 40 + dk:41 + dk],
                                                                             in1=C.h[:, dk, tc0:tc0 + 512], op0=ALU.mult, op1=ALU.add),
                         reads=[pyB, m.B] + hB(C, dk, tc0, tc0 + 512), writes=hB(C, dk, tc0, tc0 + 512))
            if ui + 2 < len(units):
                issue_w(ui + 2)
        S.barrier()


def gmlp_layer(C, m, a_w_in, a_ln_g, a_ln_b, a_w_spT, a_b_sp, a_w_out):
    nc, S = C.nc, C.S
    AI = 3072
    NJ = 24
    with ExitStack() as es:
        sb = lambda n, s, d: es.enter_context(nc.sbuf_tensor(n, s, d))
        hn = sb("ghn", [128, KC, 512], BF16)
        sq32 = sb("gsq32", [128, KC, 256], F32)
        rstd = sb("grstd", [128, 256], F32)
        uT = sb("guT", [128, NJ, 512], BF16)
        v = sb("gv", [128, 4, AI], BF16)
        lng = sb("glng", [128, AI], F32)
        lnb1 = sb("glnb1", [2, AI], F32)
        rsb = sb("grsb", [2, 8, 128], F32)
        ws32 = sb("gws32", [128, 8, 128], F32)
        wsb = sb("gwsb", [128, 8, 128], BF16)
        stats = sb("gstats", [128, 6, 6], F32)
        mv = sb("gmv", [128, 4], F32)
        wins = [sb(f"gwin{i}", [128, KC, 512], BF16) for i in range(2)]
        wouts = [sb(f"gwout{i}", [128, NJ, 128], BF16) for i in range(2)]
        cB = S.B("gconst")
        sqB, rsB = S.B("gsq"), S.B("grs")
        S.dma("sp", lambda e: e.dma_start(out=lng[:], in_=a_ln_g.partition_broadcast(128)), writes=[cB], stream="gc")
        S.op("pool", lambda e: e.memset(lnb1[:], 1.0), writes=[cB])
        S.dma("sp", lambda e: e.dma_start(out=lnb1[0:1, :], in_=a_ln_b), reads=[cB], writes=[cB], stream="gc")
        S.dma("sp", lambda e: e.dma_start(out=rsb[1:2, :, :], in_=a_b_sp), writes=[cB], stream="gc")
        S.dma("sp", lambda e: e.dma_start(out=ws32[:], in_=a_w_spT.rearrange("g s t -> s g t")), writes=[cB], stream="gc")
        S.op("pool", lambda e: e.affine_select(out=ws32[:], in_=ws32[:], pattern=[[0, 8], [1, 128]], compare_op=ALU.is_ge,
                                               fill=0.0, base=0, channel_multiplier=-1), reads=[cB], writes=[cB])
        S.op("pool", lambda e: e.tensor_copy(out=wsb[:], in_=ws32[:]), reads=[cB], writes=[cB])
        pb, pB = C.psb[5], C.pB[5]
        for half in range(2):
            S.op("pe", lambda e, half=half: e.matmul(pb[0:1, :], lhsT=C.ones32[:, 0:1], rhs=ws32[:, 4 * half:4 * half + 4, :], start=True, stop=True),
                 reads=[cB, C.cb], writes=[pB])
            S.op("act", lambda e, half=half: e.activation(out=rsb[0:1, 4 * half:4 * half + 4, :], in_=pb[0:1, :].rearrange("p (g t) -> p g t", t=128), func=AF.Copy),
                 reads=[pB], writes=[cB])
        winv = a_w_in.rearrange("(k p) n -> p k n", p=128)
        woutv = a_w_out.rearrange("(j p) d -> p j d", p=128)
        wi_n = [0]
        wo_n = [0]

        def load_win(col0):
            i = wi_n[0] % 2
            wi_n[0] += 1
            wB = S.B("gwin", i)
            S.dma("pool", lambda e: e.dma_start(out=wins[i][:], in_=winv[:, :, col0:col0 + 512]), writes=[wB], stream=f"gwin{i}")
            return wins[i], wB

        def load_wout(dk):
            i = wo_n[0] % 2
            wo_n[0] += 1
            wB = S.B("gwout", i)
            S.dma("pool", lambda e: e.dma_start(out=wouts[i][:], in_=woutv[:, :, dk * 128:(dk + 1) * 128]), writes=[wB], stream=f"gwout{i}")
            return wouts[i], wB

        hnB = lambda k: [S.B("ghn", k)]
        vB = lambda tt: S.B("gv", tt)
        uB = lambda j: S.B("guT", j)
        sh_col = lambda k: m.mod[:, 0 + k:1 + k]
        rot = 0
        for g in range(4):
            tc0 = g * 512
            for hh in range(2):
                rms_mod(C, tc0 + hh * 256, 256, m.gmod1, sh_col, m.B, sq32, sqB, rstd, rsB,
                        out_bf=lambda k, hh=hh: hn[:, k, hh * 256:(hh + 1) * 256], outB_fn=hnB)
            for s in range(6):
                w, wB = load_win(AI + s * 512)
                for tt in range(4):
                    pb, pB = C.psb[rot % 2], C.pB[rot % 2]
                    rot += 1
                    for k in range(KC):
                        S.op("pe", lambda e, k=k, tt=tt, w=w, pb=pb: e.matmul(pb[:, :], lhsT=hn[:, k, tt * 128:(tt + 1) * 128], rhs=w[:, k, :],
                                                                          start=(k == 0), stop=(k == KC - 1)),
                             reads=[wB] + hnB(k), writes=[pB], inc=(k == KC - 1))
                    S.op("act", lambda e, tt=tt, s=s, pb=pb: e.activation(out=v[:, tt, s * 512:(s + 1) * 512], in_=pb[:, :], func=AF.Gelu_apprx_tanh),
                         reads=[pB], writes=[vB(tt)])
            stB = S.B("gstats")
            for tt in range(4):
                for s in range(6):
                    S.op("dve", lambda e, tt=tt, s=s: e.bn_stats(out=stats[:, s, :], in_=v[:, tt, s * 512:(s + 1) * 512]), reads=[vB(tt)], writes=[stB])
                S.op("dve", lambda e: e.bn_aggr(out=mv[:, 0:2], in_=stats[:].rearrange("p a b -> p (a b)")), reads=[stB], writes=[stB])
                S.op("act", lambda e: e.activation(out=mv[:, 2:3], in_=mv[:, 1:2], func=AF.Sqrt, bias=C.epsc[:, 0:1], scale=1.0),
                     reads=[stB, C.cb], writes=[stB])
                S.op("dve", lambda e: e.reciprocal(out=mv[:, 2:3], in_=mv[:, 2:3]), reads=[stB], writes=[stB])
                S.op("dve", lambda e, tt=tt: e.tensor_scalar(out=v[:, tt, :], in0=v[:, tt, :], scalar1=mv[:, 0:1], scalar2=mv[:, 2:3],
                                                             op0=ALU.subtract, op1=ALU.mult), reads=[stB, vB(tt)], writes=[vB(tt)])
                S.op("dve", lambda e, tt=tt: e.tensor_tensor(out=v[:, tt, :], in0=v[:, tt, :], in1=lng[:], op=ALU.mult),
                     reads=[cB, vB(tt)], writes=[vB(tt)])
            for s in range(6):
                w, wB = load_win(s * 512)
                for jj in range(4):
                    j = 4 * s + jj
                    pb, pB = C.psb[rot % 2], C.pB[rot % 2]
                    rot += 1
                    for k in range(KC):
                        S.op("pe", lambda e, k=k, jj=jj, w=w, pb=pb: e.matmul(pb[:, :], lhsT=w[:, k, jj * 128:(jj + 1) * 128], rhs=hn[:, k, :],
                                                                          start=(k == 0), stop=(k == KC - 1)),
                             reads=[wB] + hnB(k), writes=[pB], inc=(k == KC - 1))
                    S.op("act", lambda e, j=j, pb=pb: e.activation(out=uT[:, j, :], in_=pb[:, :], func=AF.Gelu_apprx_tanh), reads=[pB], writes=[uB(j)])
            for tt in range(4):
                for jb in range(6):
                    pb, pB = C.psb[2 + rot % 2], C.pB[2 + rot % 2]
                    rot += 1
                    for jj in range(4):
                        j = 4 * jb + jj
                        gi = j // 3
                        S.op("pe", lambda e, j=j, jj=jj, gi=gi, tt=tt, pb=pb: e.matmul(pb[:, jj * 128:(jj + 1) * 128], lhsT=v[:, tt, j * 128:(j + 1) * 128],
                                                                                   rhs=wsb[:, gi, :], start=True, stop=False),
                             reads=[vB(tt), cB], writes=[pB], inc=False)
                        S.op("pe", lambda e, j=j, jj=jj, gi=gi, pb=pb: e.matmul(pb[:, jj * 128:(jj + 1) * 128], lhsT=lnb1[0:2, j * 128:(j + 1) * 128],
                                                                            rhs=rsb[0:2, gi, :], start=False, stop=True),
                             reads=[cB], writes=[pB], inc=(jj == 3))
                    uu = uT[:, 4 * jb:4 * jb + 4, tt * 128:(tt + 1) * 128]
                    S.op("dve", lambda e, uu=uu, pb=pb: e.tensor_tensor(out=uu, in0=uu, in1=pb[:, :].rearrange("p (a t) -> p a t", t=128), op=ALU.mult),
                         reads=[pB] + [uB(4 * jb + x) for x in range(4)], writes=[uB(4 * jb + x) for x in range(4)])
            for dk in range(KC):
                w, wB = load_wout(dk)
                pb, pB = C.psb[4 + dk % 2], C.pB[4 + dk % 2]
                for j in range(NJ):
                    S.op("pe", lambda e, j=j, w=w, pb=pb: e.matmul(pb[:, :], lhsT=w[:, j, :], rhs=uT[:, j, :], start=(j == 0), stop=(j == NJ - 1)),
                         reads=[wB, uB(j)], writes=[pB], inc=(j == NJ - 1))
                S.op("dve", lambda e, dk=dk, pb=pb: e.scalar_tensor_tensor(out=C.h[:, dk, tc0:tc0 + 512], in0=pb[:, :], scalar=m.mod[:, 16 + dk:17 + dk],
                                                                         in1=C.h[:, dk, tc0:tc0 + 512], op0=ALU.mult, op1=ALU.add),
                     reads=[pB, m.B] + hB(C, dk, tc0, tc0 + 512), writes=hB(C, dk, tc0, tc0 + 512))
        S.barrier()


def storage_offs(par):
    return (0, 3) if par == 0 else (1, 2)


def dsa_layer(C, m, es_outer, I, hscr, par_dummy=None):
    nc, S = C.nc, C.S
    KT = 256
    hv = hscr.rearrange("(k p) t -> p k t", p=128)
    with ExitStack() as es:
        sb = lambda n, s_, d: es.enter_context(nc.sbuf_tensor(n, s_, d))
        qT = sb("d_qT", [128, 8, NT], BF16)
        qiT = sb("d_qiT", [128, 4, NT], BF16)
        wi = sb("d_wi", [128, NTILE, 8], F32)
        qB = lambda i: S.B("d_q", i)
        bn_tm = nc.dram_tensor("bn_tm", [NT, 256], BF16).ap()
        bn_T = nc.dram_tensor("bn_T", [256, NT], BF16).ap()
        bn_ki = nc.dram_tensor("bn_ki", [64, NT], BF16).ap()
        g_tm = nc.dram_tensor("g_tm", [2 * NT, 256], BF16).ap()
        g_T = nc.dram_tensor("g_T", [512, NT], BF16).ap()
        g_ki = nc.dram_tensor("g_ki", [128, NT], BF16).ap()
        pairs = [[0, 1], [2, 3], [4, 5], [6, 7]]

        with ExitStack() as e1:
            s1 = lambda n, s_, d: e1.enter_context(nc.sbuf_tensor(n, s_, d))
            W1 = s1("d_W1", [128, KC, 1864], BF16)
            hg = s1("d_hg", [128, KC, 512], F32)
            hn = s1("d_hn", [128, KC, 512], BF16)
            sq32 = s1("d_sq32", [128, KC, 256], F32)
            rstd = s1("d_rstd", [128, 256], F32)
            stg_tm = s1("d_stg_tm", [128, NTILE, 256], BF16)
            stg_T = s1("d_stg_T", [128, 2, NT], BF16)
            stg_ki = s1("d_stg_ki", [64, NT], BF16)
            kgb = s1("d_kgb", [128, 64], F32)
            sqt = s1("d_sqt", [128, 320], F32)
            ssr = s1("d_ssr", [128, 4], F32)
            kitm = s1("d_kitm", [128, 64], BF16)
            w1B, hgB, sqB, rsB, tB = S.B("d_W1"), S.B("d_hg"), S.B("d_sq"), S.B("d_rs"), S.B("d_tmp1")
            hnB = lambda k: [S.B("d_hn", k)]
            stB = S.B("d_stg")
            w1v = I["b_w1"].rearrange("(k p) n -> p k n", p=128)
            for k in range(KC):
                S.dma("pool", lambda e, k=k: e.dma_start(out=W1[:, k, :], in_=w1v[:, k, :]), writes=[w1B], stream="d_w1")
            S.dma("sp", lambda e: e.dma_start(out=kgb[:], in_=I["kidx_g"].partition_broadcast(128)), writes=[tB], stream="d_c1")
            sh_col = lambda k: m.mod[:, k:k + 1]
            rot = 0
            for g in range(4):
                for k in range(KC):
                    S.dma("sp", lambda e, k=k: e.dma_start(out=hg[:, k, :], in_=hv[:, k, g * 512:(g + 1) * 512]),
                          reads=[S.B("hscr")], writes=[hgB], stream=f"d_hg{k % 2}")
                for hh in range(2):
                    rms_mod(C, hh * 256, 256, m.gmod1, sh_col, m.B, sq32, sqB, rstd, rsB,
                            out_bf=lambda k, hh=hh: hn[:, k, hh * 256:(hh + 1) * 256], outB_fn=hnB,
                            src=hg, srcB_fn=lambda k: [hgB])
                for ch in range(12):
                    pb, pB = C.psb[rot % 2], C.pB[rot % 2]
                    rot += 1
                    for k in range(KC):
                        S.op("pe", lambda e: e.matmul(pb[:, :], lhsT=W1[:, k, ch * 128:(ch + 1) * 128], rhs=hn[:, k, :], start=(k == 0), stop=(k == KC - 1)),
                             reads=[w1B] + hnB(k), writes=[pB], inc=(k == KC - 1))
                    dst = qT[:, ch, g * 512:(g + 1) * 512] if ch < 8 else qiT[:, ch - 8, g * 512:(g + 1) * 512]
                    wr = [qB(4 * g + x) for x in range(4)]
                    if ch % 2 == 0:
                        S.op("act", lambda e: e.activation(out=dst, in_=pb[:, :], func=AF.Copy), reads=[pB], writes=wr)
                    else:
                        S.op("dve", lambda e: e.tensor_copy(out=dst, in_=pb[:, :]), reads=[pB], writes=wr)
                for tt in range(4):
                    ti = 4 * g + tt
                    pb, pB = C.psb[2 + tt % 2], C.pB[2 + tt % 2]
                    for k in range(KC):
                        S.op("pe", lambda e: e.matmul(pb[:, 0:328], lhsT=hn[:, k, tt * 128:(tt + 1) * 128], rhs=W1[:, k, 1536:1864], start=(k == 0), stop=(k == KC - 1)),
                             reads=[w1B] + hnB(k), writes=[pB], inc=(k == KC - 1))
                    S.op("act", lambda e: e.activation(out=sqt[:], in_=pb[:, 0:320], func=AF.Square), reads=[pB], writes=[tB])
                    S.op("dve", lambda e: e.tensor_reduce(out=ssr[:, 0:1], in_=sqt[:, 0:256], axis=AX.X, op=ALU.add), reads=[tB], writes=[tB])
                    S.op("dve", lambda e: e.tensor_reduce(out=ssr[:, 1:2], in_=sqt[:, 256:320], axis=AX.X, op=ALU.add), reads=[tB], writes=[tB])
                    S.op("act", lambda e: e.activation(out=ssr[:, 2:3], in_=ssr[:, 0:1], func=AF.Sqrt, bias=C.epsc[:, 0:1], scale=1.0 / 256.0), reads=[tB, C.cb], writes=[tB])
                    S.op("act", lambda e: e.activation(out=ssr[:, 3:4], in_=ssr[:, 1:2], func=AF.Sqrt, bias=C.epsc[:, 0:1], scale=1.0 / 64.0), reads=[tB, C.cb], writes=[tB])
                    S.op("dve", lambda e: e.reciprocal(out=ssr[:, 2:4], in_=ssr[:, 2:4]), reads=[tB], writes=[tB])
                    S.op("dve", lambda e: e.tensor_scalar(out=stg_tm[:, ti, :], in0=pb[:, 0:256], scalar1=ssr[:, 2:3], scalar2=None, op0=ALU.mult),
                         reads=[pB, tB], writes=[stB])
                    S.op("dve", lambda e: e.scalar_tensor_tensor(out=kitm[:], in0=pb[:, 256:320], scalar=ssr[:, 3:4], in1=kgb[:], op0=ALU.mult, op1=ALU.mult),
                         reads=[pB, tB], writes=[tB])
                    S.op("dve", lambda e: e.tensor_scalar(out=wi[:, ti, :], in0=pb[:, 320:328], scalar1=float(8 ** -0.5 * 64 ** -0.5), scalar2=None, op0=ALU.mult),
                         reads=[pB], writes=[qB(ti)])
                    for cc in range(2):
                        S.op("pe", lambda e: e.transpose(out=C.pst[:, cc * 128:(cc + 1) * 128], in_=stg_tm[:, ti, cc * 128:(cc + 1) * 128], identity=C.identb[:]),
                             reads=[stB, C.cb], writes=[C.ptB], inc=False)
                    S.op("pe", lambda e: e.transpose(out=C.pst[0:64, 256:384], in_=kitm[:], identity=C.identb[:]), reads=[tB, C.cb], writes=[C.ptB])
                    S.op("act", lambda e: e.activation(out=stg_T[:, :, ti * 128:(ti + 1) * 128], in_=C.pst[:, 0:256].rearrange("p (c t) -> p c t", t=128), func=AF.Copy),
                         reads=[C.ptB], writes=[stB])
                    S.op("act", lambda e: e.activation(out=stg_ki[:, ti * 128:(ti + 1) * 128], in_=C.pst[0:64, 256:384], func=AF.Copy), reads=[C.ptB], writes=[stB])
            bB, gB = S.B("d_bn"), S.B("d_gath")
            S.dma("sp", lambda e: e.dma_start(out=bn_tm.rearrange("(i p) c -> p i c", p=128), in_=stg_tm[:]), reads=[stB], writes=[bB], stream="d_bn")
            S.dma("sp", lambda e: e.dma_start(out=bn_T.rearrange("(c p) t -> p c t", p=128), in_=stg_T[:]), reads=[stB], writes=[bB], stream="d_bn")
            S.dma("sp", lambda e: e.dma_start(out=bn_ki, in_=stg_ki[:]), reads=[stB], writes=[bB], stream="d_bn")
            for (bi, go) in ((bn_tm, g_tm), (bn_T, g_T), (bn_ki, g_ki)):
                S.dma("pool", lambda e: e.collective_compute("AllGather", ALU.bypass, replica_groups=pairs, ins=[bi.opt()], outs=[go.opt()]),
                      reads=[bB], writes=[gB], stream="d_cc", incv=1)
            S.barrier()

        import os
        STOP = int(os.environ.get("DSA_STOP", "9"))
        SUB = os.environ.get("DSA_SUB", "z")
        if STOP <= 1:
            return
        with ExitStack() as e3:
            s3 = lambda n, s_, d: e3.enter_context(nc.sbuf_tensor(n, s_, d))
            ckvT = s3("d_ckvT", [128, 2, SEQ], BF16)
            ckva = s3("d_ckva", [128, 32, 257], BF16)
            kiT2 = s3("d_kiT2", [128, SEQ], BF16)
            sc = s3("d_sc", [128, SEQ], F32)
            mask = s3("d_mask", [128, SEQ], BF16)
            maskT = s3("d_maskT", [128, 32, 128], BF16)
            qaT = s3("d_qaT", [128, 16, 2, 128], BF16)
            sqa = s3("d_sqa", [128, 4, 2, 128], BF16)
            olat = s3("d_olat", [128, 4, 256], BF16)
            olatT = s3("d_olatT", [128, 4, 2, 128], BF16)
            oT = s3("d_oT", [128, 8, 128], BF16)
            Hk = s3("d_Hk", [128, 2, 16, 3, 128], BF16)
            cm = s3("d_cm", [128, 2, 384], F32)
            Jb = s3("d_Jb", [128, 128], BF16)
            wukT = s3("d_wukT", [128, 8, 256], BF16)
            wuvb = s3("d_wuvb", [128, 2, 16, 64], BF16)
            wout = s3("d_wout", [128, KC, D], BF16)
            Pt = [s3(f"d_Pt{i}", [128, 4, 128], BF16) for i in range(2)]
            rl = [s3(f"d_rl{i}", [128, 512], F32) for i in range(2)]
            ytile = [s3(f"d_yt{i}", [128, KC, 128], BF16) for i in range(1)]
            kvg = s3("d_kvg", [128, 2], F32)
            gk8 = s3("d_gk8", [128, 2], F32)
            b31c = s3("d_b31c", [128, 16], F32)
            rbT = s3("d_rbT", [1, 16, 32], F32)
            bmaxr = s3("d_bmaxr", [1, 16], F32)
            rb = s3("d_rb", [32, 16], F32)
            bk = mask[0:32, 0:2048].bitcast(F32).rearrange("p (x u) -> p x u", x=2)
            vsb = mask[0:16, 2048:4096].bitcast(F32).rearrange("p (x u) -> p x u", x=2)
            shrow = s3("d_shrow", [1, 16, 128], BF16)
            sqrow = rl[0][0:1, :]
            bst = s3("d_bst", [128, 8], F32)
            rden = s3("d_rden", [128, 2], F32)
            vscr_t = nc.dram_tensor("d_vscr", [16, 2, 512], F32)
            vscr = vscr_t.ap()
            cB = S.B("d_c3")
            kvB = S.B("d_kvfull")
            gB = S.B("d_gath")
            S.dma("pool", lambda e: e.dma_start(out=wukT[:], in_=I["w_ukT"]), writes=[cB], stream="d_c3p")
            S.dma("pool", lambda e: e.dma_start(out=wout[:], in_=I["b_w_out"].rearrange("(k p) n -> p k n", p=128)), writes=[cB], stream="d_c3p")
            S.dma("sp", lambda e: e.dma_start(out=sc[:, 0:2048].rearrange("p (c h d) -> p c h d", c=2, h=16), in_=I["w_uv"].rearrange("(c p) h d -> p c h d", p=128)),
                  writes=[cB], stream="d_c3")
            S.dma("sp", lambda e: e.dma_start(out=cm[:], in_=I["cm_s"].rearrange("x t c -> t x c")), writes=[cB], stream="d_c3")
            S.dma("sp", lambda e: e.dma_start(out=kvg[:], in_=I["kvg_fm"]), writes=[cB], stream="d_c3")
            S.dma("sp", lambda e: e.dma_start(out=b31c[:], in_=I["b31"].partition_broadcast(128)), writes=[cB], stream="d_c3")
            S.dma("sp", lambda e: e.dma_start(out=rbT[:], in_=I["rel_biasT"].rearrange("o (h b) -> o h b", b=32)), writes=[cB], stream="d_c3")
            S.dma("sp", lambda e: e.dma_start(out=rb[:], in_=I["rel_bias"]), writes=[cB], stream="d_c3")
            S.dma("sp", lambda e: e.dma_start(out=bk, in_=I["bk_s"].rearrange("x b u -> b x u")), writes=[cB, S.B("d_mask")], stream="d_c3")
            for cc in range(2):
                S.op("dve", lambda e: e.tensor_scalar(out=wuvb[:, cc, :, :], in0=sc[:, cc * 1024:(cc + 1) * 1024].rearrange("p (h d) -> p h d", h=16),
                                                      scalar1=kvg[:, cc:cc + 1], scalar2=None, op0=ALU.mult), reads=[cB], writes=[cB, S.B("d_sc")])
            S.op("dve", lambda e: e.tensor_scalar(out=gk8[:], in0=kvg[:], scalar1=0.125, scalar2=None, op0=ALU.mult), reads=[cB], writes=[cB])
            S.op("dve", lambda e: e.tensor_reduce(out=bmaxr[:], in_=rbT[:], axis=AX.X, op=ALU.max), reads=[cB], writes=[cB])
            S.op("pool", lambda e: e.memset(Jb[:], 1.0), writes=[cB])
            S.op("pool", lambda e: e.affine_select(out=Jb[:], in_=Jb[:], pattern=[[1, 128]], compare_op=ALU.is_equal, fill=0.0, base=-127, channel_multiplier=1),
                 reads=[cB], writes=[cB])
            pb, pB = C.psb[6], C.pB[6]
            for x in range(2):
                S.op("pe", lambda e: e.matmul(pb[0:16, 0:512], lhsT=rb[:], rhs=bk[:, x, :], start=True, stop=True), reads=[cB], writes=[pB])
                S.op("act", lambda e: e.activation(out=vsb[:, x, :], in_=pb[0:16, 0:512], func=AF.Copy), reads=[pB], writes=[cB])
            S.dma("sp", lambda e: e.dma_start(out=vscr, in_=vsb), reads=[cB, S.B("d_mask")], writes=[S.B("d_vscr")], stream="d_c3")
            for x in range(2):
                for j in range(3):
                    src = bass.AP(vscr_t, x * 512 + 128 * (2 - j), [[1, 128], [1024, 16], [1, 128]])
                    S.dma("pool", lambda e: e.dma_start(out=Hk[:, x, :, j, :], in_=src), reads=[S.B("d_vscr")], writes=[cB], stream="d_c3p")
            for r in range(2):
                offs = storage_offs(r)
                for b in range(2):
                    off = offs[b]
                    for cc in range(2):
                        dst = ckvT[:, cc, :].rearrange("p (a q j) -> p a q j", q=4, j=128)[:, :, off, :]
                        src = g_T[r * 256 + cc * 128:r * 256 + (cc + 1) * 128, :].rearrange("p (a b j) -> p a b j", b=2, j=128)[:, :, b, :]
                        S.dma("sp", lambda e: e.dma_start(out=dst, in_=src), reads=[gB], writes=[kvB], stream="d_kv")
                    dst = ckva[:, :, 0:256].rearrange("p (a q) c -> p a q c", q=4)[:, :, off, :]
                    src = g_tm[r * NT:(r + 1) * NT, :].rearrange("(a b p) c -> p a b c", b=2, p=128)[:, :, b, :]
                    S.dma("sp", lambda e: e.dma_start(out=dst, in_=src), reads=[gB], writes=[kvB], stream="d_kv")
                    for half in range(2):
                        dst = kiT2[half * 64:(half + 1) * 64, :].rearrange("p (a q j) -> p a q j", q=4, j=128)[:, :, off, :]
                        src = g_ki[r * 64:(r + 1) * 64, :].rearrange("p (a b j) -> p a b j", b=2, j=128)[:, :, b, :]
                        S.dma("sp", lambda e: e.dma_start(out=dst, in_=src), reads=[gB], writes=[kvB], stream="d_kv")
            S.op("pool", lambda e: e.memset(ckva[:, :, 256:257], 1.0), reads=[], writes=[S.B("d_ones_col")])

            if STOP <= 2:
                S.barrier()
                return
            scB, mkB, mtB, qaB, shB, bsB = S.B("d_sc"), S.B("d_mask"), S.B("d_maskT"), S.B("d_qaT"), S.B("d_shrow"), S.B("d_bst")
            olB, oltB, oTB = S.B("d_olat"), S.B("d_olatT"), S.B("d_oT")
            lo, w0, mid, cnt, gew, hi = (bst[:, j:j + 1] for j in range(6))
            rot = 0
            prot = 0
            yrot = 0
            onescol = S.B("d_ones_col")
            for i in range(NTILE if STOP >= 9 else STOP - 2):
                X = i % 2
                nk = 4 * (i // 2) + (2 if X == 0 else 4)
                L = nk * 128
                thresh = (i >= 1)
                sp0 = max(nk - 3, 0)
                cm0 = (sp0 - (nk - 3)) * 128
                tcol = slice(i * 128, (i + 1) * 128)
                for kg in range((nk + 3) // 4):
                    c0 = kg * 512
                    W = min(512, L - c0)
                    for hi_ in range(8):
                        ch, po = hi_ // 2, (hi_ % 2) * 64
                        pb, pB = C.psb[hi_ % 2], C.pB[hi_ % 2]
                        rlt, rlB = rl[hi_ % 2], S.B("d_rl", hi_ % 2)
                        S.op("pe", lambda e: e.matmul(pb[:, 0:W], lhsT=qiT[po:po + 64, ch, tcol], rhs=kiT2[po:po + 64, c0:c0 + W], start=True, stop=True),
                             reads=[qB(i), kvB], writes=[pB])
                        S.op("act", lambda e: e.activation(out=rlt[:, 0:W], in_=pb[:, 0:W], func=AF.Relu), reads=[pB], writes=[rlB])
                        if hi_ == 0:
                            S.op("dve", lambda e: e.tensor_scalar(out=sc[:, c0:c0 + W], in0=rlt[:, 0:W], scalar1=wi[:, i, 0:1], scalar2=None, op0=ALU.mult),
                                 reads=[rlB, qB(i)], writes=[scB])
                        else:
                            S.op("dve", lambda e: e.scalar_tensor_tensor(out=sc[:, c0:c0 + W], in0=rlt[:, 0:W], scalar=wi[:, i, hi_:hi_ + 1], in1=sc[:, c0:c0 + W],
                                                                         op0=ALU.mult, op1=ALU.add), reads=[rlB, qB(i), scB], writes=[scB])
                if SUB < 'b':
                    continue
                if thresh:
                    S.op("dve", lambda e: e.tensor_reduce(out=lo, in_=sc[:, 0:L], axis=AX.X, op=ALU.min), reads=[scB], writes=[bsB])
                    S.op("dve", lambda e: e.tensor_reduce(out=hi, in_=sc[:, 0:L], axis=AX.X, op=ALU.max), reads=[scB], writes=[bsB])
                    S.op("dve", lambda e: e.tensor_tensor(out=w0, in0=hi, in1=lo, op=ALU.subtract), reads=[bsB], writes=[bsB])
                    S.op("dve", lambda e: e.tensor_scalar(out=w0, in0=w0, scalar1=1.001, scalar2=1e-6, op0=ALU.mult, op1=ALU.add), reads=[bsB], writes=[bsB])
                S.op("dve", lambda e: e.tensor_tensor(out=sc[:, sp0 * 128:L], in0=sc[:, sp0 * 128:L], in1=cm[:, X, cm0:384], op=ALU.add), reads=[scB, cB], writes=[scB])
                if thresh:
                    for it in range(1, 27):
                        f = float(2.0 ** -it)
                        S.op("dve", lambda e: e.scalar_tensor_tensor(out=mid, in0=w0, scalar=f, in1=lo, op0=ALU.mult, op1=ALU.add), reads=[bsB], writes=[bsB])
                        S.op("dve", lambda e: e.tensor_scalar(out=mask[:, 0:L], in0=sc[:, 0:L], scalar1=mid, scalar2=0.0, op0=ALU.is_ge, op1=ALU.add, accum_out=cnt),
                             reads=[scB, bsB], writes=[mkB, bsB])
                        S.op("dve", lambda e: e.tensor_scalar(out=gew, in0=cnt, scalar1=float(KT), scalar2=w0, op0=ALU.is_ge, op1=ALU.mult), reads=[bsB], writes=[bsB])
                        S.op("dve", lambda e: e.scalar_tensor_tensor(out=lo, in0=gew, scalar=f, in1=lo, op0=ALU.mult, op1=ALU.add), reads=[bsB], writes=[bsB])
                    S.op("dve", lambda e: e.tensor_scalar(out=mask[:, 0:L], in0=sc[:, 0:L], scalar1=lo, scalar2=None, op0=ALU.is_ge), reads=[scB, bsB], writes=[mkB])
                else:
                    S.op("dve", lambda e: e.tensor_scalar(out=mask[:, 0:L], in0=sc[:, 0:L], scalar1=-1e29, scalar2=None, op0=ALU.is_ge), reads=[scB], writes=[mkB])
                if SUB < 'c':
                    continue
                for k0 in range(0, nk, 8):
                    n = min(8, nk - k0)
                    for j in range(n):
                        S.op("pe", lambda e: e.transpose(out=C.pst[:, j * 128:(j + 1) * 128], in_=mask[:, (k0 + j) * 128:(k0 + j + 1) * 128], identity=C.identb[:]),
                             reads=[mkB, C.cb], writes=[C.ptB], inc=(j == n - 1))
                    S.op("act", lambda e: e.activation(out=maskT[:, k0:k0 + n, :], in_=C.pst[:, 0:n * 128].rearrange("p (a t) -> p a t", t=128), func=AF.Copy),
                         reads=[C.ptB], writes=[mtB])
                if SUB < 'd':
                    continue
                for hp2 in range(4):
                    for hh in range(2):
                        po = hh * 64
                        pb, pB = C.psb[4 + hh], C.pB[4 + hh]
                        for a in range(2):
                            hp = 2 * hp2 + a
                            for cc in range(2):
                                sl = (a * 2 + cc) * 128
                                S.op("pe", lambda e: e.matmul(pb[:, sl:sl + 128], lhsT=wukT[po:po + 64, hp, cc * 128:(cc + 1) * 128], rhs=qT[po:po + 64, hp, tcol], start=True, stop=True),
                                     reads=[cB, qB(i)], writes=[pB], inc=(a == 1 and cc == 1))
                    for hh in range(2):
                        pb, pB = C.psb[4 + hh], C.pB[4 + hh]
                        for cc in range(2):
                            src = pb[:, :].rearrange("p (a c t) -> p a c t", a=2, c=2)[:, :, cc, :]
                            h0 = 4 * hp2 + hh
                            dst = qaT[:, h0:h0 + 3:2, cc, :]
                            S.op("dve", lambda e: e.tensor_scalar(out=dst, in0=src, scalar1=gk8[:, cc:cc + 1], scalar2=None, op0=ALU.mult), reads=[pB, cB], writes=[qaB])
                if SUB < 'e':
                    continue
                for hg_ in range(4):
                    sqaB = S.B("d_sqa")
                    S.op("act", lambda e: e.activation(out=sqa[:], in_=qaT[:, 4 * hg_:4 * hg_ + 4, :, :], func=AF.Square), reads=[qaB], writes=[sqaB])
                    pb, pB = C.psb[6], C.pB[6]
                    for cc in range(2):
                        S.op("pe", lambda e: e.matmul(pb[0:1, 0:512], lhsT=C.onesb[:, 0:1], rhs=sqa[:, :, cc, :], start=(cc == 0), stop=(cc == 1)),
                             reads=[sqaB, C.cb], writes=[pB], inc=(cc == 1))
                    S.op("act", lambda e: e.activation(out=sqrow[:], in_=pb[0:1, 0:512], func=AF.Sqrt), reads=[pB], writes=[S.B("d_rl", 0)])
                    for hl in range(4):
                        h = 4 * hg_ + hl
                        S.op("dve", lambda e: e.tensor_scalar(out=shrow[0:1, h, :], in0=sqrow[0:1, hl * 128:(hl + 1) * 128], scalar1=-16.5, scalar2=bmaxr[0:1, h:h + 1],
                                                              op0=ALU.mult, op1=ALU.subtract), reads=[S.B("d_rl", 0), cB], writes=[shB])
                if SUB < 'f':
                    continue
                far = list(range(0, sp0))
                near = list(range(sp0, nk))
                batches = [far[x:x + 4] for x in range(0, len(far), 4)] + [near]
                for hg_ in range(4):
                    for hl in range(4):
                        h = 4 * hg_ + hl
                        po_, poB = C.psb[2 + h % 2], C.pB[2 + h % 2]
                        first = True
                        for bt in batches:
                            is_near = (bt is near)
                            pb, pB = C.psb[rot % 2], C.pB[rot % 2]
                            rot += 1
                            P, PB = Pt[prot % 2], S.B("d_Pt", prot % 2)
                            prot += 1
                            for j, kb in enumerate(bt):
                                ksl = slice(kb * 128, (kb + 1) * 128)
                                S.op("pe", lambda e: e.matmul(pb[:, j * 128:(j + 1) * 128], lhsT=ckvT[:, 0, ksl], rhs=qaT[:, h, 0, :], start=True, stop=False),
                                     reads=[kvB, qaB], writes=[pB], inc=False)
                                S.op("pe", lambda e: e.matmul(pb[:, j * 128:(j + 1) * 128], lhsT=ckvT[:, 1, ksl], rhs=qaT[:, h, 1, :], start=False, stop=False),
                                     reads=[kvB, qaB], writes=[pB], inc=False)
                                lastj = (j == len(bt) - 1)
                                S.op("pe", lambda e: e.matmul(pb[:, j * 128:(j + 1) * 128], lhsT=C.onesb[0:1, :], rhs=shrow[0:1, h, :], start=False, stop=(not is_near)),
                                     reads=[shB, C.cb], writes=[pB], inc=(lastj and not is_near))
                                if is_near:
                                    jj = kb - (nk - 3)
                                    S.op("pe", lambda e: e.matmul(pb[:, j * 128:(j + 1) * 128], lhsT=Jb[:], rhs=Hk[:, X, h, jj, :], start=False, stop=True),
                                         reads=[cB], writes=[pB], inc=lastj)
                            nb = len(bt)
                            pv = pb[:, 0:nb * 128]
                            Pv = P[:, 0:nb, :]
                            if is_near:
                                S.op("act", lambda e: e.activation(out=Pv, in_=pv.rearrange("p (a t) -> p a t", t=128), func=AF.Exp), reads=[pB], writes=[PB])
                            else:
                                S.op("act", lambda e: e.activation(out=Pv, in_=pv.rearrange("p (a t) -> p a t", t=128), func=AF.Exp, bias=b31c[:, h:h + 1], scale=1.0),
                                     reads=[pB, cB], writes=[PB])
                            eng = "dve" if (prot % 2 == 0) else "pool"
                            S.op(eng, lambda e: e.tensor_tensor(out=Pv, in0=Pv, in1=maskT[:, bt[0]:bt[0] + nb, :], op=ALU.mult), reads=[PB, mtB], writes=[PB])
                            for j, kb in enumerate(bt):
                                lastk = is_near and (j == nb - 1)
                                S.op("pe", lambda e: e.matmul(po_[:, 0:257], lhsT=P[:, j, :], rhs=ckva[:, kb, :], start=first, stop=lastk),
                                     reads=[PB, kvB, onescol], writes=[poB], inc=(j == nb - 1))
                                first = False
                        S.op("dve", lambda e: e.reciprocal(out=rden[:, h % 2:h % 2 + 1], in_=po_[:, 256:257]), reads=[poB], writes=[S.B("d_rden", h % 2)])
                        S.op("dve", lambda e: e.tensor_scalar(out=olat[:, hl, :], in0=po_[:, 0:256], scalar1=rden[:, h % 2:h % 2 + 1], scalar2=None, op0=ALU.mult),
                             reads=[poB, S.B("d_rden", h % 2)], writes=[olB])
                    if SUB < 'h':
                        continue
                    for hl in range(4):
                        for cc in range(2):
                            sl = (hl * 2 + cc) * 128
                            S.op("pe", lambda e: e.transpose(out=C.pst[:, sl:sl + 128], in_=olat[:, hl, cc * 128:(cc + 1) * 128], identity=C.identb[:]),
                                 reads=[olB, C.cb], writes=[C.ptB], inc=(hl == 3 and cc == 1))
                    S.op("act", lambda e: e.activation(out=olatT[:].rearrange("p a c t -> p (a c t)"), in_=C.pst[:, :], func=AF.Copy), reads=[C.ptB], writes=[oltB])
                    pb, pB = C.psb[6], C.pB[6]
                    for hl in range(4):
                        h = 4 * hg_ + hl
                        po = (h % 2) * 64
                        csl = (hl // 2) * 128
                        for cc in range(2):
                            S.op("pe", lambda e: e.matmul(pb[po:po + 64, csl:csl + 128], lhsT=wuvb[:, cc, h, :], rhs=olatT[:, hl, cc, :], start=(cc == 0), stop=(cc == 1)),
                                 reads=[cB, oltB], writes=[pB], inc=(hl == 3 and cc == 1))
                    S.op("dve", lambda e: e.tensor_copy(out=oT[:, 2 * hg_:2 * hg_ + 2, :], in_=pb[:, 0:256].rearrange("p (a t) -> p a t", t=128)), reads=[pB], writes=[oTB])
                if SUB < 'j':
                    continue
                yt, ytB = ytile[0], S.B("d_yt", 0)
                yrot += 1
                for half in range(2):
                    pb, pB = C.psb[4 + half], C.pB[4 + half]
                    for dl in range(4):
                        dk = half * 4 + dl
                        for k in range(KC):
                            S.op("pe", lambda e: e.matmul(pb[:, dl * 128:(dl + 1) * 128], lhsT=wout[:, k, dk * 128:(dk + 1) * 128], rhs=oT[:, k, :], start=(k == 0), stop=(k == KC - 1)),
                                 reads=[cB, oTB], writes=[pB], inc=(dl == 3 and k == KC - 1))
                    for dl in range(4):
                        dk = half * 4 + dl
                        S.op("dve", lambda e: e.tensor_scalar(out=yt[:, dk, :], in0=pb[:, dl * 128:(dl + 1) * 128], scalar1=m.mod[:, 16 + dk:17 + dk], scalar2=None, op0=ALU.mult),
                             reads=[pB, m.B], writes=[ytB])
                S.dma("pool", lambda e: e.dma_start(out=hv[:, :, tcol], in_=yt[:], accum_op=ALU.add), reads=[ytB, S.B("hscr")], writes=[S.B("hscr")], stream=f"d_acc{yrot % 2}")
            S.barrier()


def store_h(C, dst):
    S = C.S
    v = dst.rearrange("(k p) t -> p k t", p=128)
    toks = []
    for k in range(KC):
        toks.append(S.dma("sp", lambda e, k=k: e.dma_start(out=v[:, k, :], in_=C.h[:, k, :]),
                          reads=hB(C, k, 0, NT), writes=[S.B("out", k)], stream=f"st{k}"))
    for t in toks:
        S._wait("sp", t)


def build_layer0(do_mixer=True, do_moe=True):
    nc = bass.Bass("TRN2", target_bir_lowering=False)
    I = {}
    I["xT"] = dram_in(nc, "xT", [D, NT])
    I["cT"] = dram_in(nc, "cT", [128, KC])
    I["ada_w"] = dram_in(nc, "ada_w", [D, 6 * D])
    I["ada_bT"] = dram_in(nc, "ada_bT", [128, 48])
    I["n1g"] = dram_in(nc, "n1g", [128, KC])
    I["n2g"] = dram_in(nc, "n2g", [128, KC])
    I["a_w_in"] = dram_in(nc, "a_w_in", [D, 6144])
    I["a_ln_g"] = dram_in(nc, "a_ln_g", [1, 3072])
    I["a_ln_b"] = dram_in(nc, "a_ln_b", [1, 3072])
    I["a_w_spT"] = dram_in(nc, "a_w_spT", [8, 128, 128])
    I["a_b_sp"] = dram_in(nc, "a_b_sp", [1, 8, 128])
    I["a_w_out"] = dram_in(nc, "a_w_out", [3072, D])
    I["router_w"] = dram_in(nc, "router_w", [D, NE])
    I["router_bb"] = dram_in(nc, "router_bb", [128, NE])
    if do_moe and REPL_W:
        I["wg"] = dram_in(nc, "wg", [NE, D, DE])
        I["wu"] = dram_in(nc, "wu", [NE, D, DE])
        I["wdf"] = dram_in(nc, "wdf", [NE, DE, D])
        I["wgu"] = (I["wg"], I["wu"], I["wdf"])
        I["wd"] = None
    elif do_moe:
        I["wgu"] = dram_in(nc, "wgu", [NE, 256, DE])
        I["wd"] = dram_in(nc, "wd", [NE, 448, D])
    out = nc.dram_tensor("hT_out", [D, NT], F32, kind="ExternalOutput").ap()
    C = Ctx()
    C.nc = nc
    C.S = Sched(nc)
    with ExitStack() as es:
        setup_common(C, es)
        alloc_h(C, es)
        load_h(C, I["xT"])
        m = prologue_mod(C, es, I["cT"], I["ada_w"], I["ada_bT"], I["n1g"], I["n2g"], "0")
        if do_mixer:
            gmlp_layer(C, m, I["a_w_in"], I["a_ln_g"], I["a_ln_b"], I["a_w_spT"], I["a_b_sp"], I["a_w_out"])
        if do_moe:
            moe_layer(C, m, I["router_w"], I["router_bb"], I["wgu"], I["wd"], "0")
        store_h(C, out)
        C.S.finish()
    return nc


def final_norm_store(C, es, fg_in, dst):
    nc, S = C.nc, C.S
    with ExitStack() as e2:
        sb = lambda n, s_, d: e2.enter_context(nc.sbuf_tensor(n, s_, d))
        sq32 = sb("f_sq32", [128, KC, 256], F32)
        rstd = sb("f_rstd", [128, 256], F32)
        fg = sb("f_fg", [128, KC], F32)
        fB, sqB, rsB = S.B("f_fg"), S.B("f_sq"), S.B("f_rs")
        S.dma("sp", lambda e: e.dma_start(out=fg[:], in_=fg_in), writes=[fB], stream="f_c")
        pb, pB = C.psb[6], C.pB[6]
        v = dst.rearrange("(k p) t -> p k t", p=128)
        toks = []
        for t0 in range(0, NT, 256):
            for k in range(KC):
                S.op("act", lambda e: e.activation(out=sq32[:, k, :], in_=C.h[:, k, t0:t0 + 256], func=AF.Square), reads=hB(C, k, t0, t0 + 256), writes=[sqB])
            for k in range(KC):
                S.op("pe", lambda e: e.matmul(pb[:, 0:256], lhsT=C.ones32[:], rhs=sq32[:, k, :], start=(k == 0), stop=(k == KC - 1)),
                     reads=[sqB, C.cb], writes=[pB], inc=(k == KC - 1))
            S.op("act", lambda e: e.activation(out=rstd[:], in_=pb[:, 0:256], func=AF.Sqrt, bias=C.epsc[:, 0:1], scale=1.0 / 1024.0), reads=[pB, C.cb], writes=[rsB])
            S.op("dve", lambda e: e.reciprocal(out=rstd[:], in_=rstd[:]), reads=[rsB], writes=[rsB])
            for k in range(KC):
                S.op("dve", lambda e: e.scalar_tensor_tensor(out=C.h[:, k, t0:t0 + 256], in0=C.h[:, k, t0:t0 + 256], scalar=fg[:, k:k + 1], in1=rstd[:],
                                                             op0=ALU.mult, op1=ALU.mult), reads=hB(C, k, t0, t0 + 256) + [fB, rsB], writes=hB(C, k, t0, t0 + 256))
        for k in range(KC):
            toks.append(S.dma("sp", lambda e: e.dma_start(out=v[:, k, :], in_=C.h[:, k, :]), reads=hB(C, k, 0, NT), writes=[S.B("out", k)], stream=f"st{k}"))
        for t in toks:
            S._wait("sp", t)


L1_INPUTS = [("xT", [D, NT]), ("cT", [128, KC]), ("ada_w", [D, 6 * D]), ("ada_bT", [128, 48]), ("n1g", [128, KC]), ("n2g", [128, KC]),
             ("b_w1", [D, 1864]), ("kvg_fm", [128, 2]), ("kidx_g", [1, 64]), ("w_ukT", [128, 8, 256]), ("w_uv", [256, 16, 64]),
             ("b_w_out", [D, D]), ("rel_biasT", [1, 512]), ("rel_bias", [32, 16]), ("b31", [1, 16]), ("bk_s", [2, 32, 512]),
             ("cm_s", [2, 128, 384]), ("router_w", [D, NE]), ("router_bb", [128, NE]), ("wgu", [NE, 256, DE]), ("wd", [NE, 448, D]),
             ("final_g", [128, KC])]


MOE1_NAMES = ("xT", "cT", "ada_w", "ada_bT", "n1g", "n2g", "router_w", "router_bb", "wgu", "wd", "final_g")


def build_moe1():
    nc = bass.Bass("TRN2", target_bir_lowering=False)
    I = {n: dram_in(nc, n, shp) for n, shp in L1_INPUTS if n in MOE1_NAMES and not (REPL_W and n in ("wgu", "wd"))}
    if REPL_W:
        I["wgu"] = (dram_in(nc, "wg", [NE, D, DE]), dram_in(nc, "wu", [NE, D, DE]), dram_in(nc, "wdf", [NE, DE, D]))
        I["wd"] = None
    out = nc.dram_tensor("outT", [D, NT], F32, kind="ExternalOutput").ap()
    C = Ctx()
    C.nc = nc
    C.S = Sched(nc)
    with ExitStack() as es:
        setup_common(C, es)
        alloc_h(C, es)
        load_h(C, I["xT"])
        m = prologue_mod(C, es, I["cT"], I["ada_w"], I["ada_bT"], I["n1g"], I["n2g"], "1")
        moe_layer(C, m, I["router_w"], I["router_bb"], I["wgu"], I["wd"], "1")
        final_norm_store(C, es, I["final_g"], out)
        C.S.finish()
    return nc


def build_layer1(do_moe=True, final=True):
    nc = bass.Bass("TRN2", target_bir_lowering=False)
    I = {n: dram_in(nc, n, shp) for n, shp in L1_INPUTS if do_moe or n not in ("wgu", "wd")}
    out = nc.dram_tensor("outT", [D, NT], F32, kind="ExternalOutput").ap()
    hscr = nc.dram_tensor("hscr", [D, NT], F32).ap()
    C = Ctx()
    C.nc = nc
    C.S = Sched(nc)
    S = C.S
    with ExitStack() as es:
        setup_common(C, es)
        S.dma("sp", lambda e: e.dma_start(out=hscr, in_=I["xT"]), writes=[S.B("hscr")], stream="hcp")
        m = prologue_mod(C, es, I["cT"], I["ada_w"], I["ada_bT"], I["n1g"], I["n2g"], "1")
        dsa_layer(C, m, es, I, hscr)
        with ExitStack() as eh:
            alloc_h(C, eh, "1")
            hv = hscr.rearrange("(k p) t -> p k t", p=128)
            for k in range(KC):
                S.dma("sp", lambda e: e.dma_start(out=C.h[:, k, :], in_=hv[:, k, :]), reads=[S.B("hscr")], writes=hB(C, k, 0, NT), stream=f"ldh{k}")
            if do_moe:
                moe_layer(C, m, I["router_w"], I["router_bb"], I["wgu"], I["wd"], "1")
            if final:
                final_norm_store(C, eh, I["final_g"], out)
            else:
                store_h(C, out)
        S.finish()
    return nc


def t5_bucket_np(dist):
    n = np.maximum(dist, 0)
    nf = np.maximum(n, 1).astype(np.float32)
    large = 16 + (np.log(nf / np.float32(16)) / np.float32(np.log(128 / 16)) * np.float32(16)).astype(np.int32)
    large = np.minimum(large, 31)
    return np.where(n < 16, n, large)


def dsa_consts(par):
    bk = np.zeros((2, 32, 512), np.float32)
    cm = np.zeros((2, 128, 384), np.float32)
    u = np.arange(512)
    t = np.arange(128)[:, None]
    sidx = np.arange(128)[None, :]
    for X in range(2):
        omax = 1 if (par ^ X) == 0 else 2
        d = u - 127 + 128 * (omax - 2)
        b = t5_bucket_np(d)
        bk[X, b, u] = 1.0
        for j in range(3):
            off = omax - j
            dd = off * 128 + t - sidx
            cm[X][:, j * 128:(j + 1) * 128] = np.where(dd >= 0, 0.0, -BIG)
    return bk, cm


def layer1_inputs(inp, c, xT_core, do_moe=True):
    b = c // 2
    l = 1
    d = {}
    d["xT"] = xT_core
    d["cT"] = fm(inp["c"][b])
    d["ada_w"] = np.ascontiguousarray(inp["ada_w"][l])
    d["ada_bT"] = np.ascontiguousarray(inp["ada_b"][l].reshape(48, 128).T)
    d["n1g"] = fm(inp["norm1_g"][l])
    d["n2g"] = fm(inp["norm2_g"][l])
    w = inp["b_w_in"][0]
    d["b_w1"] = np.ascontiguousarray(np.concatenate([w[:, 0:1024], w[:, 1280:1792], w[:, 1024:1280], w[:, 1792:1856], w[:, 1856:1864]], axis=1))
    d["kvg_fm"] = np.ascontiguousarray(inp["b_kv_norm_g"][0].reshape(2, 128).T)
    d["kidx_g"] = np.ascontiguousarray(inp["b_kidx_g"][0].reshape(1, 64))
    uk = inp["b_w_uk"][0]
    d["w_ukT"] = np.ascontiguousarray(uk.reshape(256, 8, 2, 64).transpose(2, 3, 1, 0).reshape(128, 8, 256))
    d["w_uv"] = np.ascontiguousarray(inp["b_w_uv"][0])
    d["b_w_out"] = np.ascontiguousarray(inp["b_w_out"][0])
    d["rel_biasT"] = np.ascontiguousarray(inp["rel_bias"].T.reshape(1, 512))
    d["rel_bias"] = np.ascontiguousarray(inp["rel_bias"])
    d["b31"] = np.ascontiguousarray(inp["rel_bias"][31].reshape(1, 16))
    bk, cm = dsa_consts(c % 2)
    d["bk_s"] = bk
    d["cm_s"] = cm
    d["router_w"] = np.ascontiguousarray(inp["router_w"])
    d["router_bb"] = np.ascontiguousarray(np.broadcast_to(inp["router_b"].reshape(1, NE), (128, NE)))
    if do_moe:
        d.update(moe_shard(inp, l, c))
    d["final_g"] = fm(inp["final_g"])
    return d


def run_moe1(inp, h_in):
    maps = []
    for c in range(8):
        tok = core_tokens(c)
        xT = np.ascontiguousarray(h_in[c // 2][tok].T)
        mp = layer1_inputs(inp, c, xT, True)
        maps.append({k: v for k, v in mp.items() if k in MOE1_NAMES or k in ("wg", "wu", "wdf")})
    nc = build_moe1()
    res = run_bass_kernel_spmd(nc, maps, core_ids=list(range(8)))
    h = np.empty((4, SEQ, D), np.float32)
    for c in range(8):
        h[c // 2][core_tokens(c)] = res.results[c]["outT"].T
    return h, res


def run_layer1(inp, h_in, do_moe=True, trace=False, final=True):
    maps = []
    for c in range(8):
        tok = core_tokens(c)
        xT = np.ascontiguousarray(h_in[c // 2][tok].T)
        maps.append(layer1_inputs(inp, c, xT, do_moe))
    nc = build_layer1(do_moe, final)
    res = run_bass_kernel_spmd(nc, maps, core_ids=list(range(8)), trace=trace)
    h = np.empty((4, SEQ, D), np.float32)
    for c in range(8):
        h[c // 2][core_tokens(c)] = res.results[c]["outT"].T
    return h, res


def fm(vec):
    return np.ascontiguousarray(vec.reshape(KC, 128).T)


def core_tokens(c):
    par = c % 2
    qbs = my_qblocks(par)
    return np.concatenate([np.arange(q * 128, (q + 1) * 128) for q in qbs])


def layer0_inputs(inp, c, xT_core):
    b = c // 2
    l = 0
    d = {}
    d["xT"] = xT_core
    d["cT"] = fm(inp["c"][b])
    d["ada_w"] = np.ascontiguousarray(inp["ada_w"][l])
    d["ada_bT"] = np.ascontiguousarray(inp["ada_b"][l].reshape(48, 128).T)
    d["n1g"] = fm(inp["norm1_g"][l])
    d["n2g"] = fm(inp["norm2_g"][l])
    d["a_w_in"] = np.ascontiguousarray(inp["a_w_in"][0])
    d["a_ln_g"] = np.ascontiguousarray(inp["a_ln_g"][0].reshape(1, 3072))
    d["a_ln_b"] = np.ascontiguousarray(inp["a_ln_b"][0].reshape(1, 3072))
    d["a_w_spT"] = np.ascontiguousarray(inp["a_w_sp"][0].transpose(0, 2, 1))
    d["a_b_sp"] = np.ascontiguousarray(inp["a_b_sp"][0].reshape(1, 8, 128))
    d["a_w_out"] = np.ascontiguousarray(inp["a_w_out"][0])
    d["router_w"] = np.ascontiguousarray(inp["router_w"])
    d["router_bb"] = np.ascontiguousarray(np.broadcast_to(inp["router_b"].reshape(1, NE), (128, NE)))
    d.update(moe_shard(inp, l, c))
    return d


REPL_W = True


def moe_shard(inp, l, c):
    if REPL_W:
        return {"wg": np.ascontiguousarray(inp["moe_w_gate"][l]), "wu": np.ascontiguousarray(inp["moe_w_up"][l]),
                "wdf": np.ascontiguousarray(inp["moe_w_down"][l])}
    g = inp["moe_w_gate"][l][:, c * 128:(c + 1) * 128, :]
    u = inp["moe_w_up"][l][:, c * 128:(c + 1) * 128, :]
    return {"wgu": np.ascontiguousarray(np.concatenate([g, u], axis=1)),
            "wd": np.ascontiguousarray(inp["moe_w_down"][l][:, c * 448:(c + 1) * 448, :])}


def run_layer0(inp, do_mixer=True, do_moe=True, trace=False):
    x = np.asarray(inp["x"], dtype=np.float32)
    maps = []
    for c in range(8):
        tok = core_tokens(c)
        xT = np.ascontiguousarray(x[c // 2][tok].T)
        mp = layer0_inputs(inp, c, xT)
        if not do_moe:
            for kk in ("wgu", "wd", "wg", "wu", "wdf"):
                mp.pop(kk, None)
        maps.append(mp)
    nc = build_layer0(do_mixer, do_moe)
    res = run_bass_kernel_spmd(nc, maps, core_ids=list(range(8)), trace=trace)
    h = np.empty((4, SEQ, D), np.float32)
    for c in range(8):
        h[c // 2][core_tokens(c)] = res.results[c]["hT_out"].T
    return h, res


def kernel(**inputs):
    inp = {k: np.asarray(v, dtype=np.float32) for k, v in inputs.items()}
    h, _ = run_layer0(inp)
    h, _ = run_layer1(inp, h, do_moe=False, final=False)
    out, _ = run_moe1(inp, h)
    return out
```
